# Optimizing a Trainium2 kernel written in Bass

```python
import math
import jax, jax.numpy as jnp
from jax import lax
import numpy as np

D_MODEL = 1024
BATCH = 4
SEQ = 8192
DEPTH = 2

GLA_HEADS = 4
GLA_DK = 64
GLA_DV = 128
GLA_QK_DIM = GLA_HEADS * GLA_DK
GLA_V_DIM = GLA_HEADS * GLA_DV
GATE_RANK = 16
GATE_NORMALIZER = 16.0
CHUNK = 64
CONV_DIM = 512
CONV_WIDTH = 3
IN_SPLITS = (GLA_QK_DIM, GLA_QK_DIM, GLA_V_DIM, GLA_V_DIM, GATE_RANK, CONV_DIM, CONV_DIM, CONV_DIM)
IN_DIM = sum(IN_SPLITS)
MIX_DIM = GLA_V_DIM + CONV_DIM
FFN_HIDDEN = int(math.ceil(8 * D_MODEL / 3 / 256) * 256)
PLE_DIM = 256
NORM_EPS = 1e-6

kernel_name = "hybrid_gla_shortconv_parallel_heads"


def _rmsnorm(x, g):
    xf = x.astype(jnp.float32)
    xf = xf * lax.rsqrt(jnp.mean(xf * xf, axis=-1, keepdims=True) + NORM_EPS)
    return (xf * g.astype(jnp.float32)).astype(x.dtype)


def _gla_chunked(q, k, v, log_a):
    b_, s_, h_, dk = q.shape
    dv = v.shape[-1]
    n = s_ // CHUNK

    def to_chunks(t):
        return t.reshape(b_, n, CHUNK, h_, t.shape[-1]).transpose(1, 0, 3, 2, 4)

    causal = jnp.tril(jnp.ones((CHUNK, CHUNK), dtype=bool))[:, :, None]

    def step(state, inp):
        qc, kc, vc, gc = inp
        bcum = jnp.cumsum(gc, axis=2)
        diff = bcum[:, :, :, None, :] - bcum[:, :, None, :, :]
        decay = jnp.where(causal, jnp.exp(jnp.where(causal, diff, 0.0)), 0.0)
        scores = jnp.einsum('bhtd,bhsd,bhtsd->bhts', qc, kc, decay)
        o_intra = jnp.einsum('bhts,bhsv->bhtv', scores, vc)
        o_inter = jnp.einsum('bhtd,bhdv->bhtv', qc * jnp.exp(bcum), state)
        b_last = bcum[:, :, -1, :]
        k_dec = kc * jnp.exp(b_last[:, :, None, :] - bcum)
        new_state = jnp.exp(b_last)[..., None] * state + jnp.einsum('bhsd,bhsv->bhdv', k_dec, vc)
        return new_state, o_intra + o_inter

    s0 = jnp.zeros((b_, h_, dk, dv), jnp.float32)
    _, o = lax.scan(step, s0, (to_chunks(q), to_chunks(k), to_chunks(v), to_chunks(log_a)))
    return o.transpose(1, 0, 3, 2, 4).reshape(b_, s_, h_, dv)


def _causal_depthwise_conv(u, w):
    return lax.conv_general_dilated(
        u, w[:, None, :].astype(u.dtype), window_strides=(1,),
        padding=[(CONV_WIDTH - 1, 0)],
        dimension_numbers=('NWC', 'WIO', 'NWC'),
        feature_group_count=u.shape[-1])


def setup_inputs(seed: int = 0) -> dict:
    key = jax.random.key(seed)
    ks = jax.random.split(key, 20)
    nrm = lambda k, shape, scale: jax.random.normal(k, shape, jnp.float32) * scale
    gain = lambda k, shape: 1.0 + 0.05 * jax.random.normal(k, shape, jnp.float32)
    return {
        "x": nrm(ks[0], (BATCH, SEQ, D_MODEL), 1.0),
        "p": nrm(ks[1], (DEPTH, BATCH, SEQ, PLE_DIM), 1.0),
        "ln1_g": gain(ks[2], (DEPTH, D_MODEL)),
        "w_in": nrm(ks[3], (DEPTH, D_MODEL, IN_DIM), D_MODEL ** -0.5),
        "w_gate_up": nrm(ks[4], (DEPTH, GATE_RANK, GLA_QK_DIM), GATE_RANK ** -0.5),
        "b_gate": nrm(ks[5], (DEPTH, GLA_QK_DIM), 0.1),
        "conv_w": nrm(ks[6], (DEPTH, CONV_WIDTH, CONV_DIM), CONV_WIDTH ** -0.5),
        "gn_g": gain(ks[7], (DEPTH, GLA_DV)),
        "w_out": nrm(ks[8], (DEPTH, MIX_DIM, D_MODEL), MIX_DIM ** -0.5),
        "ln2_g": gain(ks[9], (DEPTH, D_MODEL)),
        "w_gate_upffn": nrm(ks[10], (DEPTH, D_MODEL, 2 * FFN_HIDDEN), D_MODEL ** -0.5),
        "w_down": nrm(ks[11], (DEPTH, FFN_HIDDEN, D_MODEL), FFN_HIDDEN ** -0.5),
        "ln3_g": gain(ks[12], (DEPTH, D_MODEL)),
        "w_ple_gate": nrm(ks[13], (DEPTH, D_MODEL, D_MODEL), D_MODEL ** -0.5),
        "w_ple_proj": nrm(ks[14], (DEPTH, PLE_DIM, D_MODEL), PLE_DIM ** -0.5),
        "lnf_g": gain(ks[15], (D_MODEL,)),
    }


def reference(x, p, ln1_g, w_in, w_gate_up, b_gate, conv_w, gn_g, w_out, ln2_g,
              w_gate_upffn, w_down, ln3_g, w_ple_gate, w_ple_proj, lnf_g):
    b_, s_, _ = x.shape
    offsets = list(np.cumsum(IN_SPLITS)[:-1])
    h = x
    for i in range(DEPTH):
        u = _rmsnorm(h, ln1_g[i])
        proj = u @ w_in[i]
        q, k, v, g, g_lr, cb, cc, cx = jnp.split(proj, offsets, axis=-1)

        gate_logits = (g_lr @ w_gate_up[i] + b_gate[i]).astype(jnp.float32)
        log_a = jax.nn.log_sigmoid(gate_logits) / GATE_NORMALIZER
        qh = q.astype(jnp.float32).reshape(b_, s_, GLA_HEADS, GLA_DK) * (GLA_DK ** -0.5)
        kh = k.astype(jnp.float32).reshape(b_, s_, GLA_HEADS, GLA_DK)
        vh = v.astype(jnp.float32).reshape(b_, s_, GLA_HEADS, GLA_DV)
        ah = log_a.reshape(b_, s_, GLA_HEADS, GLA_DK)
        o = _gla_chunked(qh, kh, vh, ah).astype(x.dtype)
        gh = g.reshape(b_, s_, GLA_HEADS, GLA_DV)
        o = _rmsnorm(o, gn_g[i]) * jax.nn.silu(gh)
        o = o.reshape(b_, s_, GLA_V_DIM)

        y = cb * _causal_depthwise_conv(cc * cx, conv_w[i])

        mix = jnp.concatenate([o, y], axis=-1) @ w_out[i]
        h = h + mix

        u2 = _rmsnorm(h, ln2_g[i])
        a, bb = jnp.split(u2 @ w_gate_upffn[i], 2, axis=-1)
        h = h + (jax.nn.silu(a) * bb) @ w_down[i]

        u3 = _rmsnorm(h, ln3_g[i])
        gate = jax.nn.sigmoid(u3 @ w_ple_gate[i])
        h = h + gate * (p[i] @ w_ple_proj[i])
    return _rmsnorm(h, lnf_g)
```

```python
from contextlib import ExitStack
import numpy as np
import concourse.bass as bass
import concourse.mybir as mybir
from concourse.bass_utils import run_bass_kernel_spmd

F32 = mybir.dt.float32
BF16 = mybir.dt.bfloat16
AF = mybir.ActivationFunctionType
ALU = mybir.AluOpType

ENGS = ("pe", "act", "dve", "pool", "sp")

D = 1024
KC = 8
SEQ = 8192
BATCH = 4
NCORE = 8
NT = 4096
T = 512
NTILE = NT // T
NBLK = T // 128
IN_DIM = 3088
FFN = 2816
PLE = 256
EPS = 1e-6
HALF_CH = (12, 10)
NVEC = 83
RUN_TILES = NTILE
OP_LIMIT = None
MARKS = []


class Buf:
    __slots__ = ("name", "w", "r", "rd", "excl", "acc")

    def __init__(self, name, excl=False):
        self.name = name
        self.w = None
        self.r = {}
        self.rd = []
        self.excl = excl
        self.acc = {}


class _Rec:
    def __init__(self):
        self.call = None

    def __getattr__(self, name):
        def f(*a, **k):
            assert self.call is None
            self.call = (name, a, k)
        return f


class Op:
    __slots__ = ("eng", "fn", "deps", "needed", "sem", "val", "is_dma")

    def __init__(self, eng, fn, is_dma):
        self.eng = eng
        rec = _Rec()
        fn(rec)
        assert rec.call is not None
        self.fn = rec.call
        self.deps = ()
        self.needed = False
        self.sem = None
        self.val = 0
        self.is_dma = is_dma


class Stream:
    __slots__ = ("sem", "count", "name", "last")

    def __init__(self, name):
        self.name = name
        self.sem = None
        self.count = 0
        self.last = None


class Prog:
    def __init__(self, nc):
        self.nc = nc
        self.ops = {e: [] for e in ENGS}
        self.streams = []
        self.nops = 0

    def stream(self, name):
        s = Stream(name)
        self.streams.append(s)
        return s

    limit = None

    def add(self, eng, fn, reads=(), writes=(), stream=None, force=False):
        if self.limit is not None and self.nops >= self.limit and not force:
            return None
        op = Op(eng, fn, stream is not None)
        deps = set()
        for b in reads:
            if not b.excl and b.w is not None:
                deps.add(b.w)
        for b in writes:
            if b.excl:
                continue
            if b.w is not None:
                deps.add(b.w)
            deps.update(b.r.values())
            deps.update(b.rd)
        for b in tuple(reads) + tuple(writes):
            if b.excl:
                for f, o in b.acc.items():
                    if f != eng:
                        deps.add(o)
                b.acc[eng] = op
        if stream is not None and stream.last is not None:
            deps.add(stream.last)
        op.deps = deps
        for b in reads:
            if b.excl:
                continue
            if op.is_dma:
                b.rd.append(op)
            else:
                b.r[eng] = op
        for b in writes:
            if b.excl:
                continue
            b.w = op
            b.r = {}
            b.rd = []
        if stream is not None:
            stream.count += 16
            stream.last = op
            op.sem = stream
            op.val = stream.count
        self.ops[eng].append(op)
        self.nops += 1
        return op

    def pe(self, fn, reads=(), writes=()):
        return self.add("pe", fn, reads, writes)

    def act(self, fn, reads=(), writes=()):
        return self.add("act", fn, reads, writes)

    def dve(self, fn, reads=(), writes=()):
        return self.add("dve", fn, reads, writes)

    def pool(self, fn, reads=(), writes=()):
        return self.add("pool", fn, reads, writes)

    def dma(self, eng, stream, fn, reads=(), writes=()):
        return self.add(eng, fn, reads, writes, stream=stream)

    def emit(self, es, final_waits=()):
        nc = self.nc
        for ops in self.ops.values():
            for op in ops:
                for d in op.deps:
                    d.needed = True
        esem = {e: es.enter_context(nc.semaphore("s_" + e)) for e in ENGS}
        for s in self.streams:
            s.sem = es.enter_context(nc.semaphore("d_" + s.name))
        for e in ENGS:
            c = 0
            for op in self.ops[e]:
                if not op.is_dma:
                    if op.needed:
                        c += 1
                    op.val = c
        block = es.enter_context(nc.Block())

        def run(e, eng):
            waited = {}
            for op in self.ops[e]:
                need = {}
                for d in op.deps:
                    if d.is_dma:
                        key = ("d", id(d.sem))
                        sem = d.sem.sem
                    else:
                        key = ("e", d.eng)
                        sem = esem[d.eng]
                    if need.get(key, (None, 0))[1] < d.val:
                        need[key] = (sem, d.val)
                for key, (sem, val) in need.items():
                    if waited.get(key, 0) < val:
                        eng.wait_ge(sem, val)
                        waited[key] = val
                name, a, k = op.fn
                ins = getattr(eng, name)(*a, **k)
                if op.is_dma:
                    ins.then_inc(op.sem.sem, 16)
                elif op.needed:
                    ins.then_inc(esem[e], 1)
            if e == "sp":
                for s in final_waits:
                    eng.wait_ge(s.sem, s.count)

        @block.tensor
        def _(eng):
            run("pe", eng)

        @block.scalar
        def _(eng):
            run("act", eng)

        @block.vector
        def _(eng):
            run("dve", eng)

        @block.gpsimd
        def _(eng):
            run("pool", eng)

        @block.sync
        def _(eng):
            run("sp", eng)


def vec_cols(l):
    base = l * 37
    return dict(ln1=base, ln2=base + 8, ln3=base + 16, gn=base + 24, cw=base + 25)


LNF_COL = 74
MASK_COL = 82


def build(phases, exch, debug=None):
    nc = bass.Bass("TRN2", target_bir_lowering=False)
    layers = sorted({l for _, l in phases})
    fused = exch == "cc"

    def din(name, shape, dt=F32):
        return nc.dram_tensor(name, list(shape), dt, kind="ExternalInput").ap()

    def dout(name, shape, dt=F32):
        return nc.dram_tensor(name, list(shape), dt, kind="ExternalOutput").ap()

    hin = din("hin", [D, NT])
    pT = din("pT", [2, PLE, NT])
    w_in = din("w_in", [2, D, IN_DIM])
    w_out = din("w_out", [2, D, D])
    w_gu = din("w_gu", [2, D, 2 * FFN])
    w_dn = din("w_dn", [2, FFN, D])
    w_pg = din("w_pg", [2, D, D])
    w_pp = din("w_pp", [2, PLE, D])
    wup_in = din("wup", [2, 17, 256])
    vecs_in = din("vecs", [128, NVEC])
    consts_in = din("consts", [128, 768])
    has_main = any(k == "main" for k, _ in phases)
    has_pre = any(k == "pre" for k, _ in phases)
    last_main = has_main and phases[-1] == ("main", 1)
    if not fused:
        if has_main:
            sx_in = din("sx_in", [128, 264])
        if has_pre:
            sx_out = dout("sx_out", [128, 264])
    if has_main:
        hout = dout("hout", [D, NT])
    dbg = {}
    if debug:
        for name, shape in debug.items():
            dbg[name] = dout("dbg_" + name, shape)

    scr = {}

    def sc(l, name, shape):
        scr[(l, name)] = nc.dram_tensor("ws%d_%s" % (l, name), list(shape), BF16).ap()

    for l in layers:
        sc(l, "qk", [128, 8, 512])
        sc(l, "v", [128, 8, 512])
        sc(l, "g", [128, 8, 512])
        sc(l, "glr", [128, 8, 16])
        for j in range(4):
            sc(l, "cv%d" % j, [128, 8, 384])
        for j in range(2):
            sc(l, "out%d" % j, [128, 8, 512])
        for j in range(11):
            sc(l, "gu%d" % j, [128, 8, 512])
        for hh in range(2):
            for cg in range(4):
                sc(l, "dn%d_%d" % (hh, cg), [128, HALF_CH[hh], 256])
        for j in range(2):
            sc(l, "pg%d" % j, [128, 8, 512])
        sc(l, "pp", [128, 2, 1024])
    if fused:
        xs_d = nc.dram_tensor("xs_d", [128, 264], F32).ap()
        xg_d = nc.dram_tensor("xg_d", [256, 264], F32).ap()

    es = ExitStack()
    with es:
        P = Prog(nc)
        P.limit = OP_LIMIT
        mark = lambda s: MARKS.append((s, P.nops))

        def sb(name, shape, dt):
            return nc.alloc_sbuf_tensor("sb_" + name, list(shape), dt)

        h = sb("h", [128, KC, NT], F32)
        hb = [[Buf("h%d_%d" % (i, m)) for m in range(KC)] for i in range(NTILE)]
        vecs = sb("vecs", [128, NVEC], F32)
        b_vecs = Buf("vecs")
        ltri = sb("ltri", [128, 128], F32)
        urev = sb("urev", [128, 128], F32)
        mask4 = sb("mask4", [128, 512], BF16)
        b_consts = Buf("consts")
        ones_d = sb("ones_d", [128, 128], BF16)
        ones_h = sb("ones_h", [128, 128], BF16)
        b_ones = Buf("ones")
        wup = sb("wupsb", [17, 2, 256], BF16)
        b_wup = Buf("wup")
        S = sb("S", [128, 2, 128], F32)
        Sbf = sb("Sbf", [128, 2, 128], BF16)
        b_S = [Buf("S0"), Buf("S1")]
        b_Sbf = [Buf("Sbf0"), Buf("Sbf1")]
        halo = sb("halo", [128, 4, 2], F32)
        b_halo = [Buf("halo%d" % j) for j in range(4)]
        sx = sb("sx", [128, 264], F32)
        b_sx = Buf("sx")
        WSLOTS = 3
        wslot = [sb("wslot%d" % i, [128, 4096], BF16) for i in range(WSLOTS)]
        b_wslot = [Buf("wslot%d" % i) for i in range(WSLOTS)]
        u = sb("u", [128, KC, T], BF16)
        b_u = Buf("u")
        NF = 3
        ftmp = [sb("ftmp%d" % i, [128, T], F32) for i in range(NF)]
        b_ftmp = [Buf("ftmp%d" % i) for i in range(NF)]
        NB = 2
        btmp = [sb("btmp%d" % i, [128, T], BF16) for i in range(NB)]
        b_btmp = [Buf("btmp%d" % i) for i in range(NB)]
        big = sb("big", [128, 12, T], BF16)
        b_big = [Buf("big%d" % j) for j in range(12)]
        mix = sb("mix", [128, 8, T], BF16)
        b_mix = [Buf("mix%d" % j) for j in range(8)]
        Epm = sb("Epm", [128, 2, 2, 128], F32)
        b_Epm = Buf("Epm")
        qblk = sb("qblk", [128, 2, 2, 128], BF16)
        b_qblk = Buf("qblk")
        ktl = sb("ktl", [128, 2, 128], BF16)
        b_ktl = Buf("ktl")
        kdec = sb("kdec", [128, 256], BF16)
        b_kdec = Buf("kdec")
        spb = sb("spb", [128, 256], F32)
        b_sp = Buf("sp")
        ebrev = sb("ebrev", [128, 256], F32)
        b_ebrev = Buf("ebrev")
        eb = sb("eb", [128, 2], F32)
        b_eb = Buf("eb")
        A_m = sb("A_m", [128, 512], BF16)
        b_Am = Buf("A_m")
        glr = sb("glr", [32, T], BF16)
        b_glr = Buf("glr")
        uc = sb("uc", [128, T + 2], F32)
        b_uc = Buf("uc")
        pTt = sb("pTt", [128, 2, T], BF16)
        b_pT = Buf("pTt")
        hx = sb("hx", [128, 8], F32)
        b_hx = Buf("hx")
        psum = [nc.alloc_psum_tensor("ps%d" % i, [128, 512], F32) for i in range(8)]
        b_ps = [Buf("ps%d" % i, excl=True) for i in range(8)]
        bank_ctr = [0]

        def bank():
            i = bank_ctr[0] % 8
            bank_ctr[0] += 1
            return psum[i], b_ps[i]

        fctr = [0]

        def ft():
            i = fctr[0] % NF
            fctr[0] += 1
            return ftmp[i], b_ftmp[i]

        bctr = [0]

        def bt():
            i = bctr[0] % NB
            bctr[0] += 1
            return btmp[i], b_btmp[i]

        s_cast = [P.stream("cast%d" % i) for i in range(6)]
        s_w = [P.stream("w%d" % i) for i in range(WSLOTS)]
        s_x = [P.stream("x%d" % i) for i in range(2)]
        s_misc = P.stream("misc")
        s_p = P.stream("p")
        s_out = [P.stream("out%d" % i) for i in range(2)]
        s_xc = P.stream("xc")

        P.dma("sp", s_misc, lambda e: e.dma_start(out=vecs[:], in_=vecs_in), writes=[b_vecs])
        P.dma("sp", s_misc, lambda e: e.dma_start(out=ltri[:], in_=consts_in[:, 0:128]), writes=[b_consts])
        P.dma("sp", s_misc, lambda e: e.dma_start(out=urev[:], in_=consts_in[:, 128:256]), writes=[b_consts])
        P.dma("sp", s_misc, lambda e: e.dma_start(out=ftmp[0][:], in_=consts_in[:, 256:768]), writes=[b_ftmp[0]])
        P.dve(lambda e: e.tensor_copy(out=mask4[:], in_=ftmp[0][:]), reads=[b_ftmp[0]], writes=[b_consts])
        P.dma("sp", s_misc, lambda e: e.dma_start(out=ftmp[1][0:17, :].rearrange("k (l c) -> k l c", l=2), in_=wup_in.rearrange("l k c -> k l c")),
              writes=[b_ftmp[1]])
        P.dve(lambda e: e.tensor_copy(out=wup[:].rearrange("k l c -> k (l c)"), in_=ftmp[1][0:17, :]), reads=[b_ftmp[1]], writes=[b_wup])
        P.pool(lambda e: e.memset(ones_d[:], 1.0 / D), writes=[b_ones])
        P.pool(lambda e: e.memset(ones_h[:], 1.0 / 128), writes=[b_ones])
        P.pool(lambda e: e.memset(glr[:], 1.0), writes=[b_glr])
        P.pool(lambda e: e.memset(qblk[:], 0.0), writes=[b_qblk])
        for i in range(NTILE):
            P.dma("sp", s_x[i % 2],
                  lambda e, i=i: e.dma_start(out=h[:, :, i * T:(i + 1) * T],
                                             in_=hin[:, i * T:(i + 1) * T].rearrange("(kc p) t -> p kc t", p=128)),
                  writes=hb[i])

        b_scr = {k: Buf("scr%d_%s" % k) for k in scr}
        cast_ctr = [0]

        pending_casts = []
        defer = [False]

        def cast(key, dst_ap, src_ap):
            def do():
                st = s_cast[cast_ctr[0] % len(s_cast)]
                cast_ctr[0] += 1
                P.dma("pool", st, lambda e: e.dma_start(out=dst_ap, in_=src_ap), writes=[b_scr[key]])
            if defer[0]:
                pending_casts.append(do)
            else:
                do()

        def drain_casts(n):
            for _ in range(min(n, len(pending_casts))):
                pending_casts.pop(0)()

        def rk(ap):
            return ap.rearrange("(kc p) c -> p kc c", p=128)

        def cast_layer(l, which):
            if which == "pre":
                cast((l, "glr"), scr[(l, "glr")], rk(w_in[l, :, 1536:1552]))
                cast((l, "qk"), scr[(l, "qk")], rk(w_in[l, :, 0:512]))
                cast((l, "v"), scr[(l, "v")], rk(w_in[l, :, 512:1024]))
                for j in range(4):
                    for q in range(3):
                        c0 = 1552 + 512 * q + 128 * j
                        cast((l, "cv%d" % j), scr[(l, "cv%d" % j)][:, :, 128 * q:128 * (q + 1)], rk(w_in[l, :, c0:c0 + 128]))
            else:
                cast((l, "g"), scr[(l, "g")], rk(w_in[l, :, 1024:1536]))
                for j in range(2):
                    cast((l, "out%d" % j), scr[(l, "out%d" % j)], rk(w_out[l, :, 512 * j:512 * (j + 1)]))
                for j in range(11):
                    cast((l, "gu%d" % j), scr[(l, "gu%d" % j)][:, :, 0:256], rk(w_gu[l, :, 256 * j:256 * (j + 1)]))
                    cast((l, "gu%d" % j), scr[(l, "gu%d" % j)][:, :, 256:512], rk(w_gu[l, :, FFN + 256 * j:FFN + 256 * (j + 1)]))
                    if j == 5 or j == 10:
                        hh = 0 if j == 5 else 1
                        r0 = 0 if hh == 0 else 12 * 128
                        nk = HALF_CH[hh]
                        for cg in range(4):
                            cast((l, "dn%d_%d" % (hh, cg)), scr[(l, "dn%d_%d" % (hh, cg))],
                                 rk(w_dn[l, r0:r0 + nk * 128, 256 * cg:256 * (cg + 1)]))
                cast((l, "pp"), scr[(l, "pp")], rk(w_pp[l, :, :]))
                for j in range(2):
                    cast((l, "pg%d" % j), scr[(l, "pg%d" % j)], rk(w_pg[l, :, 512 * j:512 * (j + 1)]))

        for li, l in enumerate(layers):
            defer[0] = li > 0
            cast_layer(l, "pre")
            cast_layer(l, "main")

        wctr = [0]

        def wload(l, name, sub=None):
            i = wctr[0] % WSLOTS
            wctr[0] += 1
            src = scr[(l, name)]
            shp = src.shape
            n = shp[1] * shp[2]
            dst = wslot[i][:, 0:n].rearrange("p (k c) -> p k c", c=shp[2])
            P.dma("sp", s_w[i], lambda e: e.dma_start(out=dst, in_=src), reads=[b_scr[(l, name)]], writes=[b_wslot[i]])
            return dst, b_wslot[i]

        def mm(out_ap, out_buf, pairs, reads):
            n = len(pairs)
            for k, (lt, rh) in enumerate(pairs):
                P.pe(lambda e, lt=lt, rh=rh, k=k: e.matmul(out_ap, lhsT=lt, rhs=rh, start=(k == 0), stop=(k == n - 1)),
                     reads=reads, writes=[out_buf])

        def norm(i, gcol, out_inplace=False):
            c0, c1 = i * T, (i + 1) * T
            ps, bps = bank()
            for kc in range(KC):
                sq, bsq = bt()
                P.act(lambda e, kc=kc, sq=sq: e.activation(out=sq[:], in_=h[:, kc, c0:c1], func=AF.Square),
                      reads=[hb[i][kc]], writes=[bsq])
                P.pe(lambda e, kc=kc, sq=sq: e.matmul(ps[:], lhsT=ones_d[:], rhs=sq[:], start=(kc == 0), stop=(kc == KC - 1)),
                     reads=[b_ones, bsq], writes=[bps])
            la, bla = ft()
            P.act(lambda e: e.activation(out=la[:], in_=ps[:], func=AF.Ln, bias=EPS), reads=[bps], writes=[bla])
            rs, brs = ft()
            P.act(lambda e: e.activation(out=rs[:], in_=la[:], func=AF.Exp, scale=-0.5), reads=[bla], writes=[brs])
            for kc in range(KC):
                if out_inplace:
                    P.dve(lambda e, kc=kc: e.scalar_tensor_tensor(out=h[:, kc, c0:c1], in0=h[:, kc, c0:c1],
                                                                  scalar=vecs[:, gcol + kc:gcol + kc + 1], in1=rs[:],
                                                                  op0=ALU.mult, op1=ALU.mult),
                          reads=[hb[i][kc], b_vecs, brs], writes=[hb[i][kc]])
                else:
                    P.dve(lambda e, kc=kc: e.scalar_tensor_tensor(out=u[:, kc, :], in0=h[:, kc, c0:c1],
                                                                  scalar=vecs[:, gcol + kc:gcol + kc + 1], in1=rs[:],
                                                                  op0=ALU.mult, op1=ALU.mult),
                          reads=[hb[i][kc], b_vecs, brs], writes=[b_u])

        def glr_proj(l):
            wg, bwg = wload(l, "glr")
            ps, bps = bank()
            mm(ps[0:16, :], bps, [(wg[:, kc, 0:16], u[:, kc, :]) for kc in range(KC)], [bwg, b_u])
            P.act(lambda e: e.activation(out=glr[0:16, :], in_=ps[0:16, :], func=AF.Copy), reads=[bps], writes=[b_glr])

        def gates_block(l, blk, full):
            t0 = blk * 128
            ps, bps = bank()
            P.pe(lambda e: e.matmul(ps[:, 0:256], lhsT=glr[0:17, t0:t0 + 128], rhs=wup[0:17, l, :], start=True, stop=True),
                 reads=[b_glr, b_wup], writes=[bps])
            P.act(lambda e: e.activation(out=spb[:], in_=ps[:, 0:256], func=AF.Exp, scale=-1.0), reads=[bps], writes=[b_sp])
            P.act(lambda e: e.activation(out=spb[:], in_=spb[:], func=AF.Ln, bias=1.0), reads=[b_sp], writes=[b_sp])
            ps2, bps2 = bank()
            P.pe(lambda e: e.matmul(ps2[:, 0:256], lhsT=urev[:], rhs=spb[:], start=True, stop=True),
                 reads=[b_consts, b_sp], writes=[bps2])
            P.act(lambda e: e.activation(out=ebrev[:], in_=ps2[:, 0:256], func=AF.Exp), reads=[bps2], writes=[b_ebrev])
            ps3, bps3 = bank()
            if full:
                for hp in range(2):
                    P.pe(lambda e, hp=hp: e.matmul(ps3[:, hp * 128:(hp + 1) * 128], lhsT=spb[:, hp * 128:(hp + 1) * 128],
                                                   rhs=ltri[:], start=True, stop=True),
                         reads=[b_consts, b_sp], writes=[bps3])
                P.act(lambda e: e.activation(out=Epm[:, 0, :, :], in_=ps3[:, 0:256].rearrange("p (a t) -> p a t", a=2), func=AF.Exp),
                      reads=[bps3], writes=[b_Epm])
                P.act(lambda e: e.activation(out=Epm[:, 1, :, :], in_=ps3[:, 0:256].rearrange("p (a t) -> p a t", a=2), func=AF.Exp, scale=-1.0),
                      reads=[bps3], writes=[b_Epm])
            else:
                for hp in range(2):
                    P.pe(lambda e, hp=hp: e.matmul(ps3[:, hp:hp + 1], lhsT=spb[:, hp * 128:(hp + 1) * 128],
                                                   rhs=ltri[:, 127:128], start=True, stop=True),
                         reads=[b_consts, b_sp], writes=[bps3])
                P.act(lambda e: e.activation(out=eb[:], in_=ps3[:, 0:2], func=AF.Exp), reads=[bps3], writes=[b_eb])

        def ktok_block(wq, bwq, blk):
            t0 = blk * 128
            ps, bps = bank()
            mm(ps[:, 0:256], bps, [(u[:, kc, t0:t0 + 128], wq[:, kc, 256:512]) for kc in range(KC)], [bwq, b_u])
            P.dve(lambda e: e.tensor_tensor(out=kdec[:], in0=ps[:, 0:256], in1=ebrev[:], op=ALU.mult),
                  reads=[bps, b_ebrev], writes=[b_kdec])

        def vtok_block(wv, bwv, blk):
            t0 = blk * 128
            ps, bps = bank()
            mm(ps[:], bps, [(u[:, kc, t0:t0 + 128], wv[:, kc, :]) for kc in range(KC)], [bwv, b_u])
            P.act(lambda e: e.activation(out=big[:, 4, :], in_=ps[:], func=AF.Copy), reads=[bps], writes=[b_big[4]])

        def state_update(decay_ap_fn, decay_buf, cast_bf):
            for hp in range(2):
                ps, bps = bank()
                P.pe(lambda e, hp=hp, ps=ps: e.matmul(ps[:, 0:256], lhsT=kdec[:, hp * 128:(hp + 1) * 128],
                                                     rhs=big[:, 4, hp * 256:(hp + 1) * 256], start=True, stop=True),
                     reads=[b_kdec, b_big[4]], writes=[bps])
                for hh in range(2):
                    p0 = hh * 64
                    P.dve(lambda e, hp=hp, hh=hh, p0=p0, ps=ps: e.scalar_tensor_tensor(
                        out=S[p0:p0 + 64, hp, :], in0=S[p0:p0 + 64, hp, :], scalar=decay_ap_fn(hp, p0),
                        in1=ps[p0:p0 + 64, hh * 128:(hh + 1) * 128], op0=ALU.mult, op1=ALU.add),
                        reads=[b_S[hp], decay_buf, bps], writes=[b_S[hp]])
                if cast_bf:
                    P.pool(lambda e, hp=hp: e.tensor_copy(out=Sbf[:, hp, :], in_=S[:, hp, :]), reads=[b_S[hp]], writes=[b_Sbf[hp]])

        def pre_tile(l, i):
            vc = vec_cols(l)
            norm(i, vc["ln1"])
            glr_proj(l)
            wq, bwq = wload(l, "qk")
            wv, bwv = wload(l, "v")
            for blk in range(NBLK):
                gates_block(l, blk, full=False)
                ktok_block(wq, bwq, blk)
                vtok_block(wv, bwv, blk)
                state_update(lambda hp, p0: eb[p0:p0 + 64, hp:hp + 1], b_eb, cast_bf=False)
            if i == RUN_TILES - 1:
                for j in range(4):
                    wc, bwc = wload(l, "cv%d" % j)
                    ps, bps = bank()
                    mm(ps[:, 0:2], bps, [(wc[:, kc, 128:256], u[:, kc, T - 2:T]) for kc in range(KC)], [bwc, b_u])
                    mm(ps[:, 2:4], bps, [(wc[:, kc, 256:384], u[:, kc, T - 2:T]) for kc in range(KC)], [bwc, b_u])
                    P.dve(lambda e, ps=ps: e.tensor_copy(out=hx[:, 0:2], in_=ps[:, 0:2]), reads=[bps], writes=[b_hx])
                    P.dve(lambda e, ps=ps, j=j: e.tensor_tensor(out=sx[:, 256 + 2 * j:258 + 2 * j], in0=hx[:, 0:2], in1=ps[:, 2:4], op=ALU.mult),
                          reads=[bps, b_hx], writes=[b_sx])

        def pre_phase(l):
            for hp in range(2):
                P.dve(lambda e, hp=hp: e.memset(S[:, hp, :], 0.0), writes=[b_S[hp]])
            for i in range(RUN_TILES):
                pre_tile(l, i)
            P.dve(lambda e: e.tensor_copy(out=sx[:, 0:256], in_=S[:].rearrange("p a b -> p (a b)")), reads=b_S, writes=[b_sx])
            if fused:
                P.dma("sp", s_xc, lambda e: e.dma_start(out=xs_d, in_=sx[:]), reads=[b_sx], writes=[b_xs])
                P.dma("pool", s_cc, lambda e: e.collective_compute(
                    "AllGather", ALU.bypass, replica_groups=[[0, 1], [2, 3], [4, 5], [6, 7]],
                    ins=[xs_d], outs=[xg_d]), reads=[b_xs], writes=[b_xg])
                P.dma("sp", s_xc, lambda e: e.dma_start(out=sx[:], in_=xg_d[0:128, :]), reads=[b_xg], writes=[b_sx])
            else:
                P.dma("sp", s_xc, lambda e: e.dma_start(out=sx_out, in_=sx[:]), reads=[b_sx])

        b_xs = Buf("xs")
        b_xg = Buf("xg")
        s_cc = P.stream("cc")

        def main_tile(l, i):
            vc = vec_cols(l)
            c0, c1 = i * T, (i + 1) * T
            mark('tile%d_start' % i)
            drain_casts(-(-92 // RUN_TILES))
            norm(i, vc["ln1"])
            glr_proj(l)
            wg, bwg = wload(l, "g")
            for hd in range(4):
                ps, bps = bank()
                mm(ps[:], bps, [(wg[:, kc, hd * 128:(hd + 1) * 128], u[:, kc, :]) for kc in range(KC)], [bwg, b_u])
                P.act(lambda e, ps=ps, hd=hd: e.activation(out=big[:, 8 + hd, :], in_=ps[:], func=AF.Silu), reads=[bps], writes=[b_big[8 + hd]])
            wq, bwq = wload(l, "qk")
            for c4 in range(4):
                ps, bps = bank()
                mm(ps[:], bps, [(wq[:, kc, c4 * 128:(c4 + 1) * 128], u[:, kc, :]) for kc in range(KC)], [bwq, b_u])
                P.act(lambda e, ps=ps, c4=c4: e.activation(out=big[:, c4, :], in_=ps[:], func=AF.Copy, scale=(0.125 if c4 < 2 else 1.0)),
                      reads=[bps], writes=[b_big[c4]])
            wv, bwv = wload(l, "v")
            mark('gla')
            for blk in range(NBLK):
                t0 = blk * 128
                mark('blk%d' % blk)
                gates_block(l, blk, full=True)
                for hp in range(2):
                    for hh in range(2):
                        p0 = hh * 64
                        P.dve(lambda e, t0=t0, hp=hp, hh=hh, p0=p0: e.tensor_tensor(
                            out=qblk[p0:p0 + 64, hp, hh, :], in0=big[p0:p0 + 64, hp, t0:t0 + 128], in1=Epm[p0:p0 + 64, 0, hp, :], op=ALU.mult),
                            reads=[b_big[hp], b_Epm], writes=[b_qblk])
                P.dve(lambda e, t0=t0: e.tensor_tensor(out=ktl[:], in0=big[:, 2:4, t0:t0 + 128], in1=Epm[:, 1, :, :], op=ALU.mult),
                      reads=[b_big[2], b_big[3], b_Epm], writes=[b_ktl])
                ktok_block(wq, bwq, blk)
                vtok_block(wv, bwv, blk)
                psA, bpsA = bank()
                for hd in range(4):
                    hp, hh = hd // 2, hd % 2
                    P.pe(lambda e, hd=hd, hp=hp, hh=hh: e.matmul(psA[:, hd * 128:(hd + 1) * 128], lhsT=ktl[:, hp, :],
                                                                rhs=qblk[:, hp, hh, :], start=True, stop=True),
                         reads=[b_ktl, b_qblk], writes=[bpsA])
                P.dve(lambda e: e.tensor_tensor(out=A_m[:], in0=psA[:], in1=mask4[:], op=ALU.mult),
                      reads=[bpsA, b_consts], writes=[b_Am])
                psO, bpsO = bank()
                for hd in range(4):
                    hp, hh = hd // 2, hd % 2
                    P.pe(lambda e, hd=hd: e.matmul(psO[:, hd * 128:(hd + 1) * 128], lhsT=big[:, 4, hd * 128:(hd + 1) * 128],
                                                   rhs=A_m[:, hd * 128:(hd + 1) * 128], start=True, stop=False),
                         reads=[b_big[4], b_Am], writes=[bpsO])
                    P.pe(lambda e, hd=hd, hp=hp, hh=hh: e.matmul(psO[:, hd * 128:(hd + 1) * 128], lhsT=Sbf[:, hp, :],
                                                                rhs=qblk[:, hp, hh, :], start=False, stop=True),
                         reads=[b_Sbf[hp], b_qblk], writes=[bpsO])
                state_update(lambda hp, p0: Epm[p0:p0 + 64, 0, hp, 127:128], b_Epm, cast_bf=True)
                osq, bosq = bt()
                P.act(lambda e, osq=osq: e.activation(out=osq[:], in_=psO[:], func=AF.Square), reads=[bpsO], writes=[bosq])
                psN, bpsN = bank()
                P.pe(lambda e, osq=osq, psN=psN: e.matmul(psN[:], lhsT=ones_h[:], rhs=osq[:], start=True, stop=True),
                     reads=[b_ones, bosq], writes=[bpsN])
                la, bla = ft()
                P.act(lambda e, la=la, psN=psN: e.activation(out=la[:], in_=psN[:], func=AF.Ln, bias=EPS), reads=[bpsN], writes=[bla])
                rs, brs = ft()
                P.act(lambda e, la=la, rs=rs: e.activation(out=rs[:], in_=la[:], func=AF.Exp, scale=-0.5), reads=[bla], writes=[brs])
                on, bon = ft()
                P.dve(lambda e, on=on, rs=rs: e.scalar_tensor_tensor(out=on[:], in0=psO[:], scalar=vecs[:, vc["gn"]:vc["gn"] + 1], in1=rs[:],
                                                                     op0=ALU.mult, op1=ALU.mult),
                      reads=[bpsO, b_vecs, brs], writes=[bon])
                P.dve(lambda e, on=on, t0=t0: e.tensor_tensor(out=mix[:, 0:4, t0:t0 + 128], in0=on[:].rearrange("p (a t) -> p a t", a=4),
                                                              in1=big[:, 8:12, t0:t0 + 128], op=ALU.mult),
                      reads=[bon] + b_big[8:12], writes=b_mix[0:4])
            mark('conv')
            for j in range(4):
                wc, bwc = wload(l, "cv%d" % j)
                psb, bpsb = bank()
                mm(psb[:], bpsb, [(wc[:, kc, 0:128], u[:, kc, :]) for kc in range(KC)], [bwc, b_u])
                psc, bpsc = bank()
                mm(psc[:], bpsc, [(wc[:, kc, 128:256], u[:, kc, :]) for kc in range(KC)], [bwc, b_u])
                psx, bpsx = bank()
                mm(psx[:], bpsx, [(wc[:, kc, 256:384], u[:, kc, :]) for kc in range(KC)], [bwc, b_u])
                cct, bcct = ft()
                P.act(lambda e, cct=cct, psc=psc: e.activation(out=cct[:], in_=psc[:], func=AF.Copy), reads=[bpsc], writes=[bcct])
                P.pool(lambda e, j=j: e.tensor_copy(out=uc[:, 0:2], in_=halo[:, j, :]), reads=[b_halo[j]], writes=[b_uc])
                P.dve(lambda e, cct=cct, psx=psx: e.tensor_tensor(out=uc[:, 2:T + 2], in0=cct[:], in1=psx[:], op=ALU.mult),
                      reads=[bcct, bpsx], writes=[b_uc])
                P.pool(lambda e, j=j: e.tensor_copy(out=halo[:, j, :], in_=uc[:, T:T + 2]), reads=[b_uc], writes=[b_halo[j]])
                y, by = ft()
                cw = vc["cw"]
                P.pool(lambda e, y=y, j=j: e.tensor_scalar(out=y[:], in0=uc[:, 0:T], scalar1=vecs[:, cw + j:cw + j + 1], scalar2=None, op0=ALU.mult),
                       reads=[b_uc, b_vecs], writes=[by])
                P.dve(lambda e, y=y, j=j: e.scalar_tensor_tensor(out=y[:], in0=uc[:, 1:T + 1], scalar=vecs[:, cw + 4 + j:cw + 5 + j], in1=y[:],
                                                                 op0=ALU.mult, op1=ALU.add),
                      reads=[b_uc, b_vecs, by], writes=[by])
                P.dve(lambda e, y=y, j=j: e.scalar_tensor_tensor(out=y[:], in0=uc[:, 2:T + 2], scalar=vecs[:, cw + 8 + j:cw + 9 + j], in1=y[:],
                                                                 op0=ALU.mult, op1=ALU.add),
                      reads=[b_uc, b_vecs, by], writes=[by])
                P.dve(lambda e, y=y, j=j, psb=psb: e.tensor_tensor(out=mix[:, 4 + j, :], in0=y[:], in1=psb[:], op=ALU.mult),
                      reads=[by, bpsb], writes=[b_mix[4 + j]])
            mark('outproj')
            for og in range(2):
                wo, bwo = wload(l, "out%d" % og)
                for mm_ in range(4):
                    m = og * 4 + mm_
                    ps, bps = bank()
                    mm(ps[:], bps, [(wo[:, kc, mm_ * 128:(mm_ + 1) * 128], mix[:, kc, :]) for kc in range(KC)], [bwo] + b_mix)
                    P.dve(lambda e, m=m, ps=ps: e.tensor_tensor(out=h[:, m, c0:c1], in0=h[:, m, c0:c1], in1=ps[:], op=ALU.add),
                          reads=[hb[i][m], bps], writes=[hb[i][m]])
            if debug and "h_mix" in debug and i == 0 and l == 0:
                P.dma("sp", s_misc, lambda e: e.dma_start(out=dbg["h_mix"].rearrange("(kc p) t -> p kc t", p=128), in_=h[:, :, 0:T]), reads=hb[0])
            mark('ffn')
            norm(i, vc["ln2"])
            jg = 0
            for hh in range(2):
                nk = HALF_CH[hh]
                for g in range(nk // 2):
                    wg, bwg = wload(l, "gu%d" % jg)
                    jg += 1
                    for q in range(2):
                        j = 2 * g + q
                        psa, bpsa = bank()
                        mm(psa[:], bpsa, [(wg[:, kc, q * 128:(q + 1) * 128], u[:, kc, :]) for kc in range(KC)], [bwg, b_u])
                        psb, bpsb = bank()
                        mm(psb[:], bpsb, [(wg[:, kc, 256 + q * 128:256 + (q + 1) * 128], u[:, kc, :]) for kc in range(KC)], [bwg, b_u])
                        st, bst = ft()
                        P.act(lambda e, st=st, psa=psa: e.activation(out=st[:], in_=psa[:], func=AF.Silu), reads=[bpsa], writes=[bst])
                        P.dve(lambda e, st=st, psb=psb, j=j: e.tensor_tensor(out=big[:, j, :], in0=st[:], in1=psb[:], op=ALU.mult),
                              reads=[bst, bpsb], writes=[b_big[j]])
                for cg in range(4):
                    wd, bwd = wload(l, "dn%d_%d" % (hh, cg))
                    for mm_ in range(2):
                        m = cg * 2 + mm_
                        ps, bps = bank()
                        mm(ps[:], bps, [(wd[:, j, mm_ * 128:(mm_ + 1) * 128], big[:, j, :]) for j in range(nk)], [bwd] + b_big[0:nk])
                        P.dve(lambda e, m=m, ps=ps: e.tensor_tensor(out=h[:, m, c0:c1], in0=h[:, m, c0:c1], in1=ps[:], op=ALU.add),
                              reads=[hb[i][m], bps], writes=[hb[i][m]])
            mark('ple')
            norm(i, vc["ln3"])
            P.dma("pool", s_p, lambda e: e.dma_start(out=pTt[:], in_=pT[l, :, c0:c1].rearrange("(kc p) t -> p kc t", p=128)), writes=[b_pT])
            wpp, bwpp = wload(l, "pp")
            for og in range(2):
                wpg, bwpg = wload(l, "pg%d" % og)
                for mm_ in range(4):
                    m = og * 4 + mm_
                    psg, bpsg = bank()
                    mm(psg[:], bpsg, [(wpg[:, kc, mm_ * 128:(mm_ + 1) * 128], u[:, kc, :]) for kc in range(KC)], [bwpg, b_u])
                    psp, bpsp = bank()
                    mm(psp[:], bpsp, [(wpp[:, k2, m * 128:(m + 1) * 128], pTt[:, k2, :]) for k2 in range(2)], [bwpp, b_pT])
                    gt, bgt = ft()
                    P.act(lambda e, gt=gt, psg=psg: e.activation(out=gt[:], in_=psg[:], func=AF.Sigmoid), reads=[bpsg], writes=[bgt])
                    P.dve(lambda e, gt=gt, psp=psp: e.tensor_tensor(out=gt[:], in0=gt[:], in1=psp[:], op=ALU.mult),
                          reads=[bgt, bpsp], writes=[bgt])
                    P.pool(lambda e, gt=gt, m=m: e.tensor_tensor(out=h[:, m, c0:c1], in0=h[:, m, c0:c1], in1=gt[:], op=ALU.add),
                           reads=[hb[i][m], bgt], writes=[hb[i][m]])
            mark('tail')
            if l == 1:
                norm(i, LNF_COL, out_inplace=True)
            if l == 1 or not fused:
                P.dma("sp", s_out[i % 2], lambda e: e.dma_start(out=hout[:, c0:c1].rearrange("(kc p) t -> p kc t", p=128), in_=h[:, :, c0:c1]),
                      reads=hb[i])

        def main_phase(l):
            if not fused:
                P.dma("sp", s_xc, lambda e: e.dma_start(out=sx[:], in_=sx_in), writes=[b_sx])
            mcol = vecs[:, MASK_COL:MASK_COL + 1]
            for hp in range(2):
                P.pool(lambda e, hp=hp: e.tensor_scalar(out=S[:, hp, :], in0=sx[:, hp * 128:(hp + 1) * 128], scalar1=mcol, scalar2=None, op0=ALU.mult),
                       reads=[b_sx, b_vecs], writes=[b_S[hp]])
                P.pool(lambda e, hp=hp: e.tensor_copy(out=Sbf[:, hp, :], in_=S[:, hp, :]), reads=[b_S[hp]], writes=[b_Sbf[hp]])
            P.pool(lambda e: e.tensor_scalar(out=halo[:].rearrange("p a b -> p (a b)"), in0=sx[:, 256:264], scalar1=mcol, scalar2=None, op0=ALU.mult),
                   reads=[b_sx, b_vecs], writes=b_halo)
            for i in range(RUN_TILES):
                main_tile(l, i)

        for kind, l in phases:
            if kind == "pre":
                pre_phase(l)
            else:
                main_phase(l)

        finals = [s_xc, s_misc]
        if has_main:
            finals += s_out
        P.emit(es, final_waits=finals)
    return nc


def _consts():
    s = np.arange(128)[:, None]
    t = np.arange(128)[None, :]
    ltri = np.where(s <= t, -1.0 / 16, 0.0).astype(np.float32)
    urev = np.where(s > t, -1.0 / 16, 0.0).astype(np.float32)
    mask = np.where(s <= t, 1.0, 0.0).astype(np.float32)
    return np.ascontiguousarray(np.concatenate([ltri, urev, mask, mask, mask, mask], axis=1))


def _vecs(core, ln1_g, ln2_g, ln3_g, gn_g, conv_w, lnf_g):
    v = np.zeros((128, NVEC), np.float32)
    for l in range(2):
        b = l * 37
        v[:, b:b + 8] = ln1_g[l].reshape(8, 128).T
        v[:, b + 8:b + 16] = ln2_g[l].reshape(8, 128).T
        v[:, b + 16:b + 24] = ln3_g[l].reshape(8, 128).T
        v[:, b + 24] = gn_g[l]
        for tap in range(3):
            v[:, b + 25 + 4 * tap:b + 29 + 4 * tap] = conv_w[l, tap].reshape(4, 128).T
    v[:, LNF_COL:LNF_COL + 8] = lnf_g.reshape(8, 128).T
    v[:, MASK_COL] = float(core % 2)
    return v


_NC_CACHE = {}


def _get_nc(phases, exch):
    key = (tuple(phases), exch)
    if key not in _NC_CACHE:
        _NC_CACHE[key] = build(list(phases), exch)
    return _NC_CACHE[key]


def _common_inputs(x, p, ln1_g, w_in, w_gate_up, b_gate, conv_w, gn_g, w_out, ln2_g,
                   w_gate_upffn, w_down, ln3_g, w_ple_gate, w_ple_proj, lnf_g):
    f = lambda a: np.ascontiguousarray(np.asarray(a, dtype=np.float32))
    wup = np.ascontiguousarray(np.concatenate([f(w_gate_up), f(b_gate)[:, None, :]], axis=1))
    consts = _consts()
    maps = []
    for c in range(NCORE):
        b, hf = c // 2, c % 2
        sl = slice(hf * NT, (hf + 1) * NT)
        maps.append({
            "pT": np.ascontiguousarray(np.transpose(f(p)[:, b, sl, :], (0, 2, 1))),
            "w_in": f(w_in), "w_out": f(w_out), "w_gu": f(w_gate_upffn), "w_dn": f(w_down),
            "w_pg": f(w_ple_gate), "w_pp": f(w_ple_proj), "wup": wup,
            "vecs": _vecs(c, f(ln1_g), f(ln2_g), f(ln3_g), f(gn_g), f(conv_w), f(lnf_g)),
            "consts": consts,
        })
    return maps


def kernel(**inputs):
    x = np.asarray(inputs["x"], dtype=np.float32)
    maps = _common_inputs(**inputs)
    hcur = []
    for c in range(NCORE):
        b, hf = c // 2, c % 2
        hcur.append(np.ascontiguousarray(x[b, hf * NT:(hf + 1) * NT, :].T))
    zeros_sx = np.zeros((128, 264), np.float32)
    cores = list(range(NCORE))

    def swap(sx):
        return [np.ascontiguousarray(sx[c - 1]) if c % 2 == 1 else zeros_sx for c in range(NCORE)]

    res = run_bass_kernel_spmd(_get_nc((("pre", 0),), "host"), [dict(m, hin=hcur[c]) for c, m in enumerate(maps)], core_ids=cores)
    sx = swap([np.asarray(r["sx_out"]) for r in res.results])
    res = run_bass_kernel_spmd(_get_nc((("main", 0), ("pre", 1)), "host"),
                               [dict(m, hin=hcur[c], sx_in=sx[c]) for c, m in enumerate(maps)], core_ids=cores)
    hcur = [np.asarray(r["hout"]) for r in res.results]
    sx = swap([np.asarray(r["sx_out"]) for r in res.results])
    res = run_bass_kernel_spmd(_get_nc((("main", 1),), "host"),
                               [dict(m, hin=hcur[c], sx_in=sx[c]) for c, m in enumerate(maps)], core_ids=cores)
    out = np.empty((BATCH, SEQ, D), np.float32)
    for c in range(NCORE):
        b, hf = c // 2, c % 2
        out[b, hf * NT:(hf + 1) * NT, :] = np.asarray(res.results[c]["hout"]).T
    return out


def kernel_unfused(**inputs):
    x = np.asarray(inputs["x"], dtype=np.float32)
    maps = _common_inputs(**inputs)
    hcur = []
    for c in range(NCORE):
        b, hf = c // 2, c % 2
        hcur.append(np.ascontiguousarray(x[b, hf * NT:(hf + 1) * NT, :].T))
    zeros_sx = np.zeros((128, 264), np.float32)
    for l in range(2):
        nc_pre = _get_nc((("pre", l),), "host")
        res = run_bass_kernel_spmd(nc_pre, [dict(m, hin=hcur[c]) for c, m in enumerate(maps)], core_ids=list(range(NCORE)))
        sx = [np.asarray(r["sx_out"]) for r in res.results]
        nc_main = _get_nc((("main", l),), "host")
        in_maps = []
        for c, m in enumerate(maps):
            sxi = sx[c - 1] if c % 2 == 1 else zeros_sx
            in_maps.append(dict(m, hin=hcur[c], sx_in=np.ascontiguousarray(sxi)))
        res = run_bass_kernel_spmd(nc_main, in_maps, core_ids=list(range(NCORE)))
        hcur = [np.asarray(r["hout"]) for r in res.results]
    out = np.empty((BATCH, SEQ, D), np.float32)
    for c in range(NCORE):
        b, hf = c // 2, c % 2
        out[b, hf * NT:(hf + 1) * NT, :] = hcur[c].T
    return out
```
